# Optimizing a Trainium2 kernel written in Bass

```python
import math
import jax
import jax.numpy as jnp
from jax import lax
import numpy as np

D_MODEL = 1024
BATCH = 1
SEQ = 16384
DEPTH = 4

CHUNK = 64
Q_BLOCK = 128
PLE_DIM = 256

A_HEADS = 8
A_HEAD_DIM = 64
A_LEFT_CHUNKS = 8
A_BAND = (A_LEFT_CHUNKS + 1) * CHUNK
A_REL_CLIP = 128
A_WIDTH = A_HEADS * A_HEAD_DIM

B_HEADS = 8
B_NOPE_DIM = 64
B_ROPE_DIM = 32
B_V_DIM = 64
B_Q_RANK = 512
B_KV_RANK = 256
ROPE_BASE = 10000.0

C_HEADS = 8
C_HEAD_DIM = 64
C_QK = 2 * C_HEADS * C_HEAD_DIM
C_OUT = C_HEADS * 2 * C_HEAD_DIM
C_IN = 2 * C_QK + C_OUT

T5_BUCKETS = 32
T5_MAX_DIST = 128

N_EXPERTS = 32
TOP_K = 4
D_FF = 1024
SWIGLU_ALPHA = 1.702
SWIGLU_LIMIT = 7.0

DEEPNORM_ALPHA = (2.0 * DEPTH) ** 0.25
DEEPNORM_BETA = (8.0 * DEPTH) ** -0.25
LN_EPS = 1e-5
RMS_EPS = 1e-6
N_AB = (DEPTH + 1) // 2
N_C = DEPTH // 2

AB_IN = 3 * A_WIDTH + B_Q_RANK + B_KV_RANK + B_ROPE_DIM
AB_OUT = A_WIDTH + B_HEADS * B_V_DIM
AB_SPLITS = [A_WIDTH, 2 * A_WIDTH, 3 * A_WIDTH, 3 * A_WIDTH + B_Q_RANK, 3 * A_WIDTH + B_Q_RANK + B_KV_RANK]
C_SPLITS = [C_QK, 2 * C_QK]

F32 = jnp.float32
NEG_INF = -1e30

kernel_name = 'hybrid_chunk_causal_moe_trunk'


def layer_norm(x, g, b):
    xf = x.astype(F32)
    mu = jnp.mean(xf, axis=-1, keepdims=True)
    var = jnp.mean(jnp.square(xf - mu), axis=-1, keepdims=True)
    return ((xf - mu) * lax.rsqrt(var + LN_EPS) * g.astype(F32) + b.astype(F32)).astype(x.dtype)


def rms_norm(x, g):
    xf = x.astype(F32)
    inv = lax.rsqrt(jnp.mean(jnp.square(xf), axis=-1, keepdims=True) + RMS_EPS)
    return (xf * inv * g.astype(F32)).astype(x.dtype)


def rope(x, pos):
    d = x.shape[-1]
    inv_freq = ROPE_BASE ** (-jnp.arange(0, d, 2, dtype=F32) / d)
    ang = pos.astype(F32)[:, None] * inv_freq[None, :]
    cos = jnp.cos(ang)[None, :, None, :]
    sin = jnp.sin(ang)[None, :, None, :]
    xf = x.astype(F32)
    x1, x2 = xf[..., : d // 2], xf[..., d // 2:]
    return jnp.concatenate([x1 * cos - x2 * sin, x2 * cos + x1 * sin], axis=-1).astype(x.dtype)


def masked_softmax(logits, mask):
    return jax.nn.softmax(jnp.where(mask, logits, NEG_INF), axis=-1)


def block_chunk_mask(block_idx, s):
    q_pos = block_idx * Q_BLOCK + jnp.arange(Q_BLOCK)
    k_pos = jnp.arange(s)
    return (k_pos[None, :] // CHUNK) <= (q_pos[:, None] // CHUNK)


def sweep_query_blocks(block_fn, q_arrays):
    b, s = q_arrays[0].shape[:2]
    nb = s // Q_BLOCK
    xs = tuple(jnp.swapaxes(q.reshape(b, nb, Q_BLOCK, *q.shape[2:]), 0, 1) for q in q_arrays)
    out = lax.map(lambda args: block_fn(args[0], *args[1:]), (jnp.arange(nb), *xs))
    return jnp.swapaxes(out, 0, 1).reshape(b, s, *out.shape[3:])


def t5_bucket(rel):
    half = T5_BUCKETS // 2
    max_exact = half // 2
    ret = jnp.where(rel > 0, half, 0)
    n = jnp.abs(rel)
    large = max_exact + (jnp.log(jnp.maximum(n, max_exact).astype(F32) / max_exact)
                         / math.log(T5_MAX_DIST / max_exact) * (half - max_exact)).astype(jnp.int32)
    large = jnp.minimum(large, half - 1)
    return ret + jnp.where(n < max_exact, n, large)


def chunked_band_attention(q, k, v, rel_bias):
    b, s, h, dh = q.shape
    nc = s // CHUNK
    qc = q.reshape(b, nc, CHUNK, h, dh)
    pad = ((0, 0), (A_LEFT_CHUNKS, 0), (0, 0), (0, 0), (0, 0))
    kc = jnp.pad(k.reshape(b, nc, CHUNK, h, dh), pad)
    vc = jnp.pad(v.reshape(b, nc, CHUNK, h, dh), pad)
    k_band = jnp.concatenate([kc[:, j:j + nc] for j in range(A_LEFT_CHUNKS + 1)], axis=2)
    v_band = jnp.concatenate([vc[:, j:j + nc] for j in range(A_LEFT_CHUNKS + 1)], axis=2)
    logits = jnp.einsum('bcqhd,bckhd->bchqk', qc, k_band).astype(F32) * (dh ** -0.5)
    qi = jnp.arange(CHUNK)
    ki = jnp.arange(A_BAND)
    rel = A_LEFT_CHUNKS * CHUNK + qi[:, None] - ki[None, :]
    bias = rel_bias[jnp.clip(rel, -A_REL_CLIP, A_REL_CLIP) + A_REL_CLIP]
    logits = logits + jnp.transpose(bias, (2, 0, 1)).astype(F32)[None, None]
    valid = (jnp.arange(nc)[:, None] - A_LEFT_CHUNKS + ki[None, :] // CHUNK) >= 0
    probs = masked_softmax(logits, valid[None, :, None, None, :])
    out = jnp.einsum('bchqk,bckhd->bcqhd', probs.astype(v.dtype), v_band)
    return out.reshape(b, s, h, dh)


def latent_attention(c_q, c_kv, k_r, q_norm_g, kv_norm_g, w_uq, w_ukv, pos):
    b, s, _ = c_q.shape
    q = (rms_norm(c_q, q_norm_g) @ w_uq).reshape(b, s, B_HEADS, B_NOPE_DIM + B_ROPE_DIM)
    q_nope = q[..., :B_NOPE_DIM]
    q_rope = rope(q[..., B_NOPE_DIM:], pos)
    kv = (rms_norm(c_kv, kv_norm_g) @ w_ukv).reshape(b, s, B_HEADS, B_NOPE_DIM + B_V_DIM)
    k_nope = kv[..., :B_NOPE_DIM]
    v = kv[..., B_NOPE_DIM:]
    k_rope = rope(k_r[:, :, None, :], pos)[:, :, 0]
    scale = (B_NOPE_DIM + B_ROPE_DIM) ** -0.5

    def block(bi, qn, qr):
        logits = (jnp.einsum('bqhd,bkhd->bhqk', qn, k_nope).astype(F32)
                  + jnp.einsum('bqhd,bkd->bhqk', qr, k_rope).astype(F32)) * scale
        probs = masked_softmax(logits, block_chunk_mask(bi, s)[None, None])
        return jnp.einsum('bhqk,bkhd->bqhd', probs.astype(v.dtype), v)

    return sweep_query_blocks(block, (q_nope, q_rope))


def lambda_init(layer_idx):
    return 0.8 - 0.6 * math.exp(-0.3 * layer_idx)


def differential_attention(q, k, v, lam_params, subln_g, t5_table, lam_init):
    b, s = q.shape[:2]
    lp = lam_params.astype(F32)
    lam = jnp.exp(jnp.sum(lp[0] * lp[1])) - jnp.exp(jnp.sum(lp[2] * lp[3])) + lam_init
    scale = C_HEAD_DIM ** -0.5

    def block(bi, qb):
        logits = jnp.einsum('bqhnd,bkhnd->bhnqk', qb, k).astype(F32) * scale
        rel = jnp.arange(s)[None, :] - (bi * Q_BLOCK + jnp.arange(Q_BLOCK))[:, None]
        bias = jnp.transpose(t5_table[t5_bucket(rel)], (2, 0, 1)).astype(F32)
        probs = masked_softmax(logits + bias[None, :, None], block_chunk_mask(bi, s)[None, None, None])
        attn = probs[:, :, 0] - lam * probs[:, :, 1]
        return jnp.einsum('bhqk,bkhd->bqhd', attn.astype(v.dtype), v)

    o = sweep_query_blocks(block, (q,))
    return rms_norm(o, subln_g) * (1.0 - lam_init)


def moe_ffn(x, w_router, b_router, w_in, b_in, w_out, b_out):
    logits = (x @ w_router).astype(F32) + b_router.astype(F32)
    top_val, top_idx = lax.top_k(logits, TOP_K)
    top_w = jax.nn.softmax(top_val, axis=-1)
    gates = jnp.einsum('bsk,bske->ebs', top_w, jax.nn.one_hot(top_idx, N_EXPERTS, dtype=F32))

    def expert(acc, params):
        wi, bi, wo, bo, g = params
        h = x @ wi + bi
        glu = jnp.minimum(h[..., :D_FF], SWIGLU_LIMIT)
        lin = jnp.clip(h[..., D_FF:], -SWIGLU_LIMIT, SWIGLU_LIMIT)
        y = (glu * jax.nn.sigmoid(SWIGLU_ALPHA * glu) * (lin + 1.0)) @ wo + bo
        return acc + g[..., None].astype(x.dtype) * y, None

    out, _ = lax.scan(expert, jnp.zeros_like(x), (w_in, b_in, w_out, b_out, gates))
    return out


def _normal(key, shape, scale):
    return jax.random.normal(key, shape, F32) * scale


def setup_inputs(seed: int = 0) -> dict:
    key = jax.random.key(seed)
    ks = jax.random.split(key, 26)
    d = D_MODEL
    return {
        'x': _normal(ks[0], (BATCH, SEQ, d), 1.0),
        'p': _normal(ks[1], (DEPTH, BATCH, SEQ, PLE_DIM), 1.0),
        'ab_w_in': _normal(ks[2], (N_AB, d, AB_IN), d ** -0.5),
        'ab_rel_bias': _normal(ks[3], (N_AB, 2 * A_REL_CLIP + 1, A_HEADS), 0.2),
        'ab_q_norm': 1.0 + _normal(ks[4], (N_AB, B_Q_RANK), 0.01),
        'ab_kv_norm': 1.0 + _normal(ks[5], (N_AB, B_KV_RANK), 0.01),
        'ab_w_uq': _normal(ks[6], (N_AB, B_Q_RANK, B_HEADS * (B_NOPE_DIM + B_ROPE_DIM)), B_Q_RANK ** -0.5),
        'ab_w_ukv': _normal(ks[7], (N_AB, B_KV_RANK, B_HEADS * (B_NOPE_DIM + B_V_DIM)), B_KV_RANK ** -0.5),
        'ab_w_out': _normal(ks[8], (N_AB, AB_OUT, d), DEEPNORM_BETA * AB_OUT ** -0.5),
        'c_w_in': _normal(ks[9], (N_C, d, C_IN), d ** -0.5),
        'c_lambda': _normal(ks[10], (N_C, 4, C_HEAD_DIM), 0.1),
        'c_subln': 1.0 + _normal(ks[11], (N_C, 2 * C_HEAD_DIM), 0.01),
        'c_w_out': _normal(ks[12], (N_C, C_OUT, d), DEEPNORM_BETA * C_OUT ** -0.5),
        't5_table': _normal(ks[13], (T5_BUCKETS, C_HEADS), 0.2),
        'ln_mix_g': 1.0 + _normal(ks[14], (DEPTH, d), 0.01),
        'ln_mix_b': _normal(ks[15], (DEPTH, d), 0.01),
        'ln_ffn_g': 1.0 + _normal(ks[16], (DEPTH, d), 0.01),
        'ln_ffn_b': _normal(ks[17], (DEPTH, d), 0.01),
        'router_w': _normal(ks[18], (DEPTH, d, N_EXPERTS), d ** -0.5),
        'router_b': _normal(ks[19], (DEPTH, N_EXPERTS), 0.01),
        'exp_w_in': _normal(ks[20], (DEPTH, N_EXPERTS, d, 2 * D_FF), d ** -0.5),
        'exp_b_in': _normal(ks[21], (DEPTH, N_EXPERTS, 2 * D_FF), 0.01),
        'exp_w_out': _normal(ks[22], (DEPTH, N_EXPERTS, D_FF, d), DEEPNORM_BETA * D_FF ** -0.5),
        'exp_b_out': _normal(ks[23], (DEPTH, N_EXPERTS, d), 0.01),
        'ple_gate_w': _normal(ks[24], (DEPTH, d, d), d ** -0.5),
        'ple_proj_w': _normal(ks[25], (DEPTH, PLE_DIM, d), DEEPNORM_BETA * PLE_DIM ** -0.5),
    }


def reference(x, p, ab_w_in, ab_rel_bias, ab_q_norm, ab_kv_norm, ab_w_uq, ab_w_ukv, ab_w_out,
              c_w_in, c_lambda, c_subln, c_w_out, t5_table,
              ln_mix_g, ln_mix_b, ln_ffn_g, ln_ffn_b,
              router_w, router_b, exp_w_in, exp_b_in, exp_w_out, exp_b_out,
              ple_gate_w, ple_proj_w):
    b, s, _ = x.shape
    pos = jnp.arange(s)
    for i in range(DEPTH):
        j = i // 2
        if i % 2 == 0:
            h = x @ ab_w_in[j]
            qa, ka, va, c_q, c_kv, k_r = jnp.split(h, AB_SPLITS, axis=-1)
            o_a = chunked_band_attention(qa.reshape(b, s, A_HEADS, A_HEAD_DIM),
                                         ka.reshape(b, s, A_HEADS, A_HEAD_DIM),
                                         va.reshape(b, s, A_HEADS, A_HEAD_DIM),
                                         ab_rel_bias[j])
            o_b = latent_attention(c_q, c_kv, k_r, ab_q_norm[j], ab_kv_norm[j],
                                   ab_w_uq[j], ab_w_ukv[j], pos)
            mixed = jnp.concatenate([o_a.reshape(b, s, -1), o_b.reshape(b, s, -1)], axis=-1) @ ab_w_out[j]
        else:
            h = x @ c_w_in[j]
            qc, kc, vc = jnp.split(h, C_SPLITS, axis=-1)
            o_c = differential_attention(qc.reshape(b, s, C_HEADS, 2, C_HEAD_DIM),
                                         kc.reshape(b, s, C_HEADS, 2, C_HEAD_DIM),
                                         vc.reshape(b, s, C_HEADS, 2 * C_HEAD_DIM),
                                         c_lambda[j], c_subln[j], t5_table, lambda_init(i))
            mixed = o_c.reshape(b, s, C_OUT) @ c_w_out[j]
        x = layer_norm(DEEPNORM_ALPHA * x + mixed, ln_mix_g[i], ln_mix_b[i])
        ffn = moe_ffn(x, router_w[i], router_b[i], exp_w_in[i], exp_b_in[i], exp_w_out[i], exp_b_out[i])
        ple = jax.nn.sigmoid(x @ ple_gate_w[i]) * (p[i] @ ple_proj_w[i])
        x = layer_norm(DEEPNORM_ALPHA * x + ffn + ple, ln_ffn_g[i], ln_ffn_b[i])
    return x
```

```python
import math
from contextlib import ExitStack

import numpy as np
import ml_dtypes
import concourse.bass as bass
import concourse.mybir as mybir
from concourse.bass_utils import run_bass_kernel_spmd

F32 = mybir.dt.float32
BF16 = mybir.dt.bfloat16
U8 = mybir.dt.uint8
ALU = mybir.AluOpType
AF = mybir.ActivationFunctionType
AX = mybir.AxisListType
NPBF = ml_dtypes.bfloat16

NCORES = 8
D = 1024
SEQ = 16384
T = SEQ // NCORES
DEPTH = 4
NE = 32
DFF = 1024
PLE = 256
ALPHA = (2.0 * DEPTH) ** 0.25
LN_EPS = 1e-5
RMS_EPS = 1e-6
NEG = -30000.0


def _size(dt):
    return {F32: 4, BF16: 2, U8: 1}[dt]


class Sched:
    CH = 30000
    ENG = ("sp", "act", "dve", "pool", "pe")

    def __init__(self):
        self.streams = {e: [] for e in self.ENG}
        self.cnt = {e: 0 for e in self.ENG}
        self.dcnt = {}
        self.last_w = {}
        self.readers = {}
        self.waited = {e: {} for e in self.ENG}
        self.semkeys = []

    def _tok(self, engine, dma):
        if dma is None:
            n = self.cnt[engine]
            self.cnt[engine] = n + 1
            key = (engine, n // self.CH)
            val = n % self.CH + 1
            inc = 1
        else:
            key = ("dma", dma)
            n = self.dcnt.get(dma, 0) + 1
            self.dcnt[dma] = n
            val = 16 * n
            inc = 16
        if key not in self.semkeys:
            self.semkeys.append(key)
        return (key, val, engine if dma is None else None), inc

    def op(self, engine, fn, reads=(), writes=(), dma=None):
        deps = {}
        for r in reads:
            t = self.last_w.get(r)
            if t is not None:
                deps[t] = True
        for w in writes:
            t = self.last_w.get(w)
            if t is not None:
                deps.setdefault(t, False)
            for t in self.readers.get(w, ()):
                deps.setdefault(t, False)
        waits = []
        wd = self.waited[engine]
        for (key, val, eng), raw in deps.items():
            if eng == engine and not raw:
                continue
            if wd.get(key, 0) >= val:
                continue
            wd[key] = val
            waits.append((key, val))
        tok, inc = self._tok(engine, dma)
        for r in reads:
            self.readers.setdefault(r, []).append(tok)
        for w in writes:
            self.last_w[w] = tok
            self.readers[w] = []
        self.streams[engine].append((waits, fn, (tok[0], inc)))
        return tok

    def barrier(self):
        allt = []
        for e in self.ENG:
            n = self.cnt[e]
            if n:
                allt.append(((e, (n - 1) // self.CH), (n - 1) % self.CH + 1))
        for k, n in self.dcnt.items():
            allt.append((("dma", k), 16 * n))
        for e in self.ENG:
            waits = []
            wd = self.waited[e]
            for key, val in allt:
                if wd.get(key, 0) >= val:
                    continue
                wd[key] = val
                waits.append((key, val))
            if waits:
                self.streams[e].append((waits, None, None))

    def emit(self, nc):
        with ExitStack() as st:
            sems = {}
            for i, k in enumerate(self.semkeys):
                sems[k] = st.enter_context(nc.semaphore("s%d" % i))
            with nc.Block() as block:
                def run(eng, E):
                    for waits, fn, sig in self.streams[E]:
                        for k, v in waits:
                            eng.wait_ge(sems[k], v)
                        if fn is None:
                            continue
                        ins = fn(eng)
                        ins.then_inc(sems[sig[0]], sig[1])

                @block.sync
                def _(e):
                    run(e, "sp")

                @block.scalar
                def _(e):
                    run(e, "act")

                @block.vector
                def _(e):
                    run(e, "dve")

                @block.gpsimd
                def _(e):
                    run(e, "pool")

                @block.tensor
                def _(e):
                    run(e, "pe")


class KB:
    def __init__(self, arena_bytes=206 * 1024):
        self.nc = bass.Bass("TRN2", target_bir_lowering=False)
        self.S = Sched()
        self.st = ExitStack()
        self.arena = self.st.enter_context(self.nc.sbuf_tensor("arena", [128, arena_bytes], U8))
        self.arena_bytes = arena_bytes
        self.off = 0
        self.ps = [self.st.enter_context(self.nc.psum_tensor("ps%d" % i, [128, 512], F32))[:, :] for i in range(8)]
        self.psi = 0
        self.uid = 0
        self.qi = 0
        self.ins = {}
        self.outs = {}

    def din(self, name, shape, dt=F32):
        ap = self.nc.dram_tensor(name, list(shape), dt, kind="ExternalInput").ap()
        self.ins[name] = ap
        return ap

    def dout(self, name, shape, dt=F32):
        ap = self.nc.dram_tensor(name, list(shape), dt, kind="ExternalOutput").ap()
        self.outs[name] = ap
        return ap

    def alloc(self, free_shape, dt, name=None):
        n = int(np.prod(free_shape)) * _size(dt)
        n_al = (n + 63) // 64 * 64
        assert self.off + n_al <= self.arena_bytes, ("SBUF arena overflow", name, self.off, n_al)
        v = self.arena[:, self.off:self.off + n].bitcast(dt)
        self.off += n_al
        if len(free_shape) == 2:
            v = v.rearrange("p (a b) -> p a b", a=free_shape[0])
        elif len(free_shape) == 3:
            v = v.rearrange("p (a b c) -> p a b c", a=free_shape[0], b=free_shape[1])
        return v

    def mark(self):
        return self.off

    def release(self, m):
        self.off = m

    def name(self, p="r"):
        self.uid += 1
        return "%s%d" % (p, self.uid)

    def bank(self):
        i = self.psi
        self.psi = (self.psi + 1) % 8
        return self.ps[i], "ps%d" % i

    def q(self):
        self.qi += 1
        return ("sp", "pool")[self.qi % 2]

    def dma(self, out, in_, reads, writes, key, q=None):
        self.S.op(q or "sp", lambda e: e.dma_start(out=out, in_=in_), reads, writes, dma=key)

    def mm(self, out, pairs, reads, writes, start=True, stop=True):
        def fn(e):
            n = len(pairs)
            for i, (l, r) in enumerate(pairs):
                ins = e.matmul(out, lhsT=l, rhs=r, start=(start and i == 0), stop=(stop and i == n - 1))
            return ins
        self.S.op("pe", fn, reads, writes)

    def tr(self, out, in_, ident, reads, writes):
        self.S.op("pe", lambda e: e.transpose(out=out, in_=in_, identity=ident), reads, writes)

    def act(self, out, in_, func, reads, writes, bias=None, scale=None):
        kw = {}
        if bias is not None:
            kw["bias"] = bias
        if scale is not None:
            kw["scale"] = scale
        self.S.op("act", lambda e: e.activation(out=out, in_=in_, func=func, **kw), reads, writes)

    def ts(self, eng, out, in0, s1, s2, op0, op1, reads, writes):
        if s2 is None:
            self.S.op(eng, lambda e: e.tensor_single_scalar(out=out, in_=in0, scalar=s1, op=op0), reads, writes)
        else:
            self.S.op(eng, lambda e: e.tensor_scalar(out=out, in0=in0, scalar1=s1, scalar2=s2, op0=op0, op1=op1), reads, writes)

    def tt(self, eng, out, in0, in1, op, reads, writes):
        self.S.op(eng, lambda e: e.tensor_tensor(out=out, in0=in0, in1=in1, op=op), reads, writes)

    def stt(self, eng, out, in0, scalar, in1, op0, op1, reads, writes):
        self.S.op(eng, lambda e: e.scalar_tensor_tensor(out=out, in0=in0, scalar=scalar, in1=in1, op0=op0, op1=op1), reads, writes)

    def cp(self, eng, out, in_, reads, writes):
        if eng == "act":
            self.S.op(eng, lambda e: e.copy(out=out, in_=in_), reads, writes)
        else:
            self.S.op(eng, lambda e: e.tensor_copy(out=out, in_=in_), reads, writes)

    def memset(self, eng, out, val, writes):
        self.S.op(eng, lambda e: e.memset(out, val), (), writes)

    def consts(self):
        self.ident = self.alloc([128], BF16, "ident")
        self.ones = self.alloc([128], BF16, "ones")
        self.memset("pool", self.ident, 0.0, ["ident"])
        self.S.op("pool", lambda e: e.affine_select(out=self.ident, in_=self.ident, pattern=[[-1, 128]],
                                                  compare_op=ALU.not_equal, fill=1.0, base=0, channel_multiplier=1),
                  ["ident"], ["ident"])
        self.memset("pool", self.ones, 1.0, ["ones"])
        self.eps_rms = self.alloc([1], F32)
        self.eps_ln = self.alloc([1], F32)
        self.memset("pool", self.eps_rms, RMS_EPS, ["eps"])
        self.memset("pool", self.eps_ln, LN_EPS, ["eps"])

    def rstd(self, out, in_, scale, eps, reads, writes):
        self.act(out, in_, AF.Sqrt, list(reads) + ["eps"], writes, bias=eps[0:out.shape[0], 0:1], scale=scale)
        self.S.op("dve", lambda e: e.reciprocal(out=out, in_=out), writes, writes)

    def load_cast(self, dst_bf, src_dram, shape_free, stg, stg_names, idx, dst_name, eng="dve", q=None):
        s = idx % len(stg)
        sv = stg[s]
        for d in shape_free[::-1]:
            pass
        n = int(np.prod(shape_free))
        v = sv[:, 0:n]
        if len(shape_free) == 2:
            v = v.rearrange("p (a b) -> p a b", a=shape_free[0])
        self.dma(v, src_dram, [], [stg_names[s]], stg_names[s], q=q)
        self.cp(eng, dst_bf, v, [stg_names[s]], [dst_name])

    def finish(self):
        self.S.barrier()
        self.S.emit(self.nc)
        self.st.close()
        return self.nc


def run(kb, in_maps):
    nc = kb.finish()
    res = run_bass_kernel_spmd(nc, in_maps, core_ids=list(range(NCORES)))
    return res.results


def emit_x2xt(kb, x_tok_dram, xt_dram, ntok):
    m = kb.mark()
    xs = [kb.alloc([1024], F32) for _ in range(2)]
    xb = [kb.alloc([1024], BF16) for _ in range(2)]
    xt = [kb.alloc([8, 128], BF16) for _ in range(2)]
    for s in range(ntok // 128):
        k = s % 2
        kb.dma(xs[k], x_tok_dram[s * 128:(s + 1) * 128, :], [], ["x2xs%d" % k], "x2xs%d" % k)
        kb.cp("dve", xb[k], xs[k], ["x2xs%d" % k], ["x2xb%d" % k])
        pb, pn = kb.bank()
        pbv = pb.bitcast(BF16)
        for c in range(8):
            kb.tr(pbv[:, c * 128:(c + 1) * 128], xb[k][:, c * 128:(c + 1) * 128], kb.ident, ["x2xb%d" % k, "ident"], [pn])
        kb.cp("act", xt[k], pbv[:, 0:1024].rearrange("p (c t) -> p c t", c=8), [pn], ["x2xt%d" % k])
        kb.dma(xt_dram.rearrange("(c p) t -> p c t", p=128)[:, :, s * 128:(s + 1) * 128], xt[k], ["x2xt%d" % k], [], "x2xt%d" % k, q="pool")
    kb.release(m)


def emit_pre_even(kb, d):
    m = kb.mark()
    stg = [kb.alloc([2368], F32) for _ in range(2)]
    stn = ["pstg0", "pstg1"]
    W1 = kb.alloc([8, 2368], BF16)
    WA = kb.alloc([4, 768], BF16)
    WB = kb.alloc([4, 768], BF16)
    WK = kb.alloc([2, 512], BF16)
    WV = kb.alloc([2, 512], BF16)
    gq = kb.alloc([4], F32)
    gkv = kb.alloc([2], F32)
    li = 0
    for c in range(8):
        kb.load_cast(W1[:, c, :], d["w1"][c * 128:(c + 1) * 128, :], [2368], stg, stn, li, "W1", eng=("dve", "pool")[li % 2]); li += 1
    for c in range(4):
        kb.load_cast(WA[:, c, :], d["wa"][c * 128:(c + 1) * 128, :], [768], stg, stn, li, "WA", eng=("dve", "pool")[li % 2]); li += 1
        kb.load_cast(WB[:, c, :], d["wb"][c * 128:(c + 1) * 128, :], [768], stg, stn, li, "WB", eng=("dve", "pool")[li % 2]); li += 1
    for c in range(2):
        kb.load_cast(WK[:, c, :], d["wk"][c * 128:(c + 1) * 128, :], [512], stg, stn, li, "WK", eng=("dve", "pool")[li % 2]); li += 1
        kb.load_cast(WV[:, c, :], d["wv"][c * 128:(c + 1) * 128, :], [512], stg, stn, li, "WV", eng=("dve", "pool")[li % 2]); li += 1
    kb.dma(gq, d["gq"], [], ["gq"], "gq")
    kb.dma(gkv, d["gkv"], [], ["gkv"], "gkv")

    XT = [kb.alloc([8, 512], BF16) for _ in range(2)]
    CQ = [kb.alloc([512], F32) for _ in range(2)]
    SQ = [kb.alloc([512], F32) for _ in range(2)]
    CK = [kb.alloc([512], F32) for _ in range(2)]
    SK = [kb.alloc([512], F32) for _ in range(2)]
    cqg = kb.alloc([4, 512], BF16)
    sqq = kb.alloc([4, 512], BF16)
    ckvg = kb.alloc([2, 512], BF16)
    sqkv = kb.alloc([2, 512], BF16)
    invq = kb.alloc([512], F32)
    invkv = kb.alloc([512], F32)
    invc = kb.alloc([4], F32)
    CI = kb.alloc([512], F32)
    SI = kb.alloc([512], F32)
    NO = 4
    ob = [kb.alloc([512], BF16) for _ in range(NO)]
    t1 = [kb.alloc([512], F32) for _ in range(2)]
    t2 = [kb.alloc([512], F32) for _ in range(2)]
    oi = [0]
    ti = [0]

    def outslot():
        k = oi[0] % NO
        oi[0] += 1
        return ob[k], "pob%d" % k

    xtv = d["xt"].rearrange("(c p) t -> p c t", p=128)
    for t in range(T // 512):
        k = t % 2
        tok = slice(t * 512, (t + 1) * 512)
        xn = "pxt%d" % k
        kb.dma(XT[k], xtv[:, :, tok], [], [xn], xn)
        kb.dma(CQ[k][0:96, :], d["cq"][:, tok], [], ["pcq%d" % k], "pcq%d" % k, q="pool")
        kb.dma(SQ[k][0:96, :], d["sq"][:, tok], [], ["psq%d" % k], "psq%d" % k, q="pool")
        kb.dma(CK[k][0:32, :], d["ck"][:, tok], [], ["pck%d" % k], "pck%d" % k, q="pool")
        kb.dma(SK[k][0:32, :], d["sk"][:, tok], [], ["psk%d" % k], "psk%d" % k, q="pool")
        X = XT[k]

        def fm(col0, mcols):
            pb, pn = kb.bank()
            kb.mm(pb[0:mcols, :], [(W1[:, c, col0:col0 + mcols], X[:, c, :]) for c in range(8)], ["W1", xn], [pn])
            return pb, pn

        for nm, base in (("qa", 0), ("ka", 512)):
            for j in range(4):
                pb, pn = fm(base + j * 128, 128)
                o, on = outslot()
                kb.cp(("act", "dve")[j % 2], o, pb, [pn], [on])
                kb.dma(d[nm][j * 128:(j + 1) * 128, tok], o, [on], [], on, q=kb.q())
        for s in range(4):
            pb, pn = kb.bank()
            kb.mm(pb, [(X[:, c, s * 128:(s + 1) * 128], W1[:, c, 1024:1536]) for c in range(8)], ["W1", xn], [pn])
            o, on = outslot()
            kb.cp(("act", "dve")[s % 2], o, pb, [pn], [on])
            kb.dma(d["va"][t * 512 + s * 128:t * 512 + (s + 1) * 128, :], o, [on], [], on, q=kb.q())
        for j in range(4):
            pb, pn = fm(1536 + j * 128, 128)
            kb.act(cqg[:, j, :], pb, AF.Copy, [pn, "gq"], ["cqg"], scale=gq[:, j:j + 1])
            kb.act(sqq[:, j, :], pb, AF.Square, [pn], ["sqq"])
        pb, pn = kb.bank()
        kb.mm(pb, [(kb.ones, sqq[:, j, :]) for j in range(4)], ["ones", "sqq"], [pn])
        kb.rstd(invq, pb, 1.0 / 512, kb.eps_rms, [pn], ["invq"])
        kb.tt("pool", CI[0:96, :], CQ[k][0:96, :], invq[0:96, :], ALU.mult, ["pcq%d" % k, "invq"], ["CI"])
        kb.tt("pool", SI[0:96, :], SQ[k][0:96, :], invq[0:96, :], ALU.mult, ["psq%d" % k, "invq"], ["SI"])
        for j in range(2):
            pb, pn = fm(2048 + j * 128, 128)
            kb.act(ckvg[:, j, :], pb, AF.Copy, [pn, "gkv"], ["ckvg"], scale=gkv[:, j:j + 1])
            kb.act(sqkv[:, j, :], pb, AF.Square, [pn], ["sqkv"])
        pb, pn = kb.bank()
        kb.mm(pb, [(kb.ones, sqkv[:, j, :]) for j in range(2)], ["ones", "sqkv"], [pn])
        kb.rstd(invkv, pb, 1.0 / 256, kb.eps_rms, [pn], ["invkv"])
        pb, pn = kb.bank()
        for s in range(4):
            kb.mm(pb[:, s:s + 1], [(sqkv[:, j, s * 128:(s + 1) * 128], kb.ones[:, 0:1]) for j in range(2)], ["ones", "sqkv"], [pn])
        kb.rstd(invc, pb[:, 0:4], 1.0 / 256, kb.eps_rms, [pn], ["invc"])
        pb1, pn1 = fm(2304, 32)
        pb2, pn2 = fm(2336, 32)
        a = ti[0] % 2; ti[0] += 1
        kb.tt("dve", t1[a][0:32, :], pb1[0:32, :], CK[k][0:32, :], ALU.mult, [pn1, "pck%d" % k], ["pt1%d" % a])
        kb.tt("dve", t2[a][0:32, :], pb2[0:32, :], SK[k][0:32, :], ALU.mult, [pn2, "psk%d" % k], ["pt2%d" % a])
        o, on = outslot()
        kb.tt("pool", o[0:32, :], t1[a][0:32, :], t2[a][0:32, :], ALU.add, ["pt1%d" % a, "pt2%d" % a], [on])
        for h in range(8):
            kb.dma(d["km"][h * 96 + 64:h * 96 + 96, tok], o[0:32, :], [on], [], on, q=kb.q())
        for h in range(8):
            pa, pna = kb.bank()
            kb.mm(pa[0:96, :], [(WA[:, c, h * 96:(h + 1) * 96], cqg[:, c, :]) for c in range(4)], ["WA", "cqg"], [pna])
            pbb, pnb = kb.bank()
            kb.mm(pbb[0:96, :], [(WB[:, c, h * 96:(h + 1) * 96], cqg[:, c, :]) for c in range(4)], ["WB", "cqg"], [pnb])
            a = ti[0] % 2; ti[0] += 1
            kb.tt("dve", t1[a][0:96, :], pa[0:96, :], CI[0:96, :], ALU.mult, [pna, "CI"], ["pt1%d" % a])
            kb.tt("dve", t2[a][0:96, :], pbb[0:96, :], SI[0:96, :], ALU.mult, [pnb, "SI"], ["pt2%d" % a])
            o, on = outslot()
            kb.tt("pool", o[0:96, :], t1[a][0:96, :], t2[a][0:96, :], ALU.add, ["pt1%d" % a, "pt2%d" % a], [on])
            kb.dma(d["qm"][h * 96:(h + 1) * 96, tok], o[0:96, :], [on], [], on, q=kb.q())
        for jj in range(4):
            pb, pn = kb.bank()
            kb.mm(pb, [(WK[:, c, jj * 128:(jj + 1) * 128], ckvg[:, c, :]) for c in range(2)], ["WK", "ckvg"], [pn])
            o, on = outslot()
            kb.tt("dve", o, pb, invkv, ALU.mult, [pn, "invkv"], [on])
            kb.dma(d["km"][(2 * jj) * 96:(2 * jj) * 96 + 64, tok], o[0:64, :], [on], [], on, q=kb.q())
            kb.dma(d["km"][(2 * jj + 1) * 96:(2 * jj + 1) * 96 + 64, tok], o[64:128, :], [on], [], on, q=kb.q())
        for s in range(4):
            pb, pn = kb.bank()
            kb.mm(pb, [(ckvg[:, c, s * 128:(s + 1) * 128], WV[:, c, :]) for c in range(2)], ["WV", "ckvg"], [pn])
            o, on = outslot()
            kb.act(o, pb, AF.Copy, [pn, "invc"], [on], scale=invc[:, s:s + 1])
            kb.dma(d["vm"][t * 512 + s * 128:t * 512 + (s + 1) * 128, :], o, [on], [], on, q=kb.q())
    kb.release(m)


def emit_pre_odd(kb, d):
    m = kb.mark()
    stg = [kb.alloc([3072], F32) for _ in range(2)]
    stn = ["ostg0", "ostg1"]
    W = kb.alloc([8, 3072], BF16)
    for c in range(8):
        kb.load_cast(W[:, c, :], d["w"][c * 128:(c + 1) * 128, :], [3072], stg, stn, c, "WC", eng=("dve", "pool")[c % 2])
    XT = [kb.alloc([8, 512], BF16) for _ in range(2)]
    NO = 4
    ob = [kb.alloc([512], BF16) for _ in range(NO)]
    oi = 0
    xtv = d["xt"].rearrange("(c p) t -> p c t", p=128)
    for t in range(T // 512):
        k = t % 2
        tok = slice(t * 512, (t + 1) * 512)
        xn = "oxt%d" % k
        kb.dma(XT[k], xtv[:, :, tok], [], [xn], xn)
        X = XT[k]
        for nm, base in (("qc", 0), ("kc", 1024)):
            for j in range(8):
                pb, pn = kb.bank()
                kb.mm(pb, [(W[:, c, base + j * 128:base + (j + 1) * 128], X[:, c, :]) for c in range(8)], ["WC", xn], [pn])
                o, on = ob[oi % NO], "oob%d" % (oi % NO); oi += 1
                kb.cp(("act", "dve")[j % 2], o, pb, [pn], [on])
                kb.dma(d[nm][j * 128:(j + 1) * 128, tok], o, [on], [], on, q=kb.q())
        for s in range(4):
            for hf in range(2):
                pb, pn = kb.bank()
                kb.mm(pb, [(X[:, c, s * 128:(s + 1) * 128], W[:, c, 2048 + hf * 512:2048 + (hf + 1) * 512]) for c in range(8)], ["WC", xn], [pn])
                o, on = ob[oi % NO], "oob%d" % (oi % NO); oi += 1
                kb.cp(("act", "dve")[hf % 2], o, pb, [pn], [on])
                kb.dma(d["vc"][t * 512 + s * 128:t * 512 + (s + 1) * 128, hf * 512:(hf + 1) * 512], o, [on], [], on, q=kb.q())
    kb.release(m)


def rope_tables(pos):
    inv_freq = (10000.0 ** (-np.arange(0, 32, 2, dtype=np.float32) / 32)).astype(np.float32)
    ang = pos.astype(np.float32)[:, None] * inv_freq[None, :]
    cos = np.cos(ang).astype(np.float32).T
    sin = np.sin(ang).astype(np.float32).T
    n = pos.shape[0]
    cq = np.concatenate([np.ones((64, n), np.float32), cos, cos], 0)
    sq = np.concatenate([np.zeros((64, n), np.float32), -sin, sin], 0)
    ck = np.concatenate([cos, cos], 0)
    sk = np.concatenate([-sin, sin], 0)
    return [np.ascontiguousarray(a) for a in (cq, sq, ck, sk)]


def prep_even(ab_w_in, ab_q_norm, ab_kv_norm, ab_w_uq, ab_w_ukv):
    w = ab_w_in
    kr = w[:, 2304:2336]
    w1 = np.concatenate([w, kr[:, 16:32], kr[:, 0:16]], 1)
    uq = ab_w_uq.reshape(512, 8, 96)
    wb = np.concatenate([uq[:, :, 0:64], uq[:, :, 80:96], uq[:, :, 64:80]], 2).reshape(512, 768)
    ukv = ab_w_ukv.reshape(256, 8, 128)
    wk = ukv[:, :, 0:64].reshape(256, 512)
    wv = ukv[:, :, 64:128].reshape(256, 512)
    return dict(w1=np.ascontiguousarray(w1), wa=np.ascontiguousarray(ab_w_uq), wb=np.ascontiguousarray(wb),
                wk=np.ascontiguousarray(wk), wv=np.ascontiguousarray(wv),
                gq=np.ascontiguousarray(ab_q_norm.reshape(4, 128).T), gkv=np.ascontiguousarray(ab_kv_norm.reshape(2, 128).T))


def declare_pre_even(kb):
    d = {}
    d["xt"] = kb.din("xt", [1024, T], BF16)
    d["w1"] = kb.din("w1", [1024, 2368])
    d["wa"] = kb.din("wa", [512, 768])
    d["wb"] = kb.din("wb", [512, 768])
    d["wk"] = kb.din("wk", [256, 512])
    d["wv"] = kb.din("wv", [256, 512])
    d["gq"] = kb.din("gq", [128, 4])
    d["gkv"] = kb.din("gkv", [128, 2])
    d["cq"] = kb.din("cq", [96, T])
    d["sq"] = kb.din("sq", [96, T])
    d["ck"] = kb.din("ck", [32, T])
    d["sk"] = kb.din("sk", [32, T])
    for nm, shp in (("qa", [512, T]), ("ka", [512, T]), ("va", [T, 512]), ("qm", [768, T]), ("km", [768, T]), ("vm", [T, 512])):
        d[nm] = kb.dout(nm, shp, BF16)
    return d


class AttnState:
    def __init__(self, kb):
        self.si = 0
        self.pi = 0
        self.ti = 0
        self.P = [kb.alloc([512], BF16) for _ in range(3)]
        self.TMP = [kb.alloc([512], F32) for _ in range(2)]


def attn_block(kb, st, q_ap, kT, V, Dk, Dv, kp0, scale, klist, btiles, cbias, Ob, Lb, rd):
    n = len(klist)
    Op, On = kb.ps[Ob], "ps%d" % Ob
    Lp, Ln = kb.ps[Lb], "ps%d" % Lb
    sb = []

    def qk(i):
        b = st.si % 3
        st.si += 1
        kbi = klist[i][0]
        kb.mm(kb.ps[b], [(kT[kp0:kp0 + Dk, kbi * 128:(kbi + 1) * 128], q_ap)], rd, ["ps%d" % b])
        sb.append(b)

    qk(0)
    for i in range(n):
        if i + 1 < n:
            qk(i + 1)
        b = sb[i]
        kbi, bt = klist[i]
        p = st.pi % 3
        st.pi += 1
        Pn = "attP%d" % p
        if bt is None:
            kb.act(st.P[p], kb.ps[b], AF.Exp, ["ps%d" % b] + (["cbias"] if cbias is not None else []), [Pn],
                   bias=cbias, scale=scale)
        else:
            t = st.ti % 2
            st.ti += 1
            kb.stt("dve", st.TMP[t], kb.ps[b], scale, btiles[:, bt, :], ALU.mult, ALU.add, ["ps%d" % b, "btiles"], ["attT%d" % t])
            kb.act(st.P[p], st.TMP[t], AF.Exp, ["attT%d" % t], [Pn])
        kb.mm(Op[0:Dv, :], [(V[:, kbi, 0:Dv], st.P[p])], [Pn, "attV"], [On], start=(i == 0), stop=(i == n - 1))
        kb.mm(Lp[0:Dv, :], [(kb.ones[:, 0:Dv], st.P[p])], [Pn, "ones"], [Ln], start=(i == 0), stop=(i == n - 1))


def load_qkv(kb, qT, kT, V, q_d, k_d, v_d, Dk2, Dv):
    for i in range(4):
        cs = slice(i * 4096, (i + 1) * 4096)
        kb.dma(qT[0:Dk2, cs], q_d[:, cs], [], ["attQ"], "attQ", q="sp")
        kb.dma(kT[0:Dk2, cs], k_d[:, cs], [], ["attK"], "attK", q="pool")
    vv = v_d.rearrange("(b p) d -> p b d", p=128)
    for i in range(4):
        kb.dma(V[:, i * 32:(i + 1) * 32, 0:Dv], vv[:, i * 32:(i + 1) * 32, :], [], ["attV"], "attV", q=("sp", "pool")[i % 2])


def emit_attn_even(kb, d):
    m = kb.mark()
    qT = kb.alloc([SEQ], BF16)
    kT = kb.alloc([SEQ], BF16)
    V = kb.alloc([128, 64], BF16)
    bt = kb.alloc([8, 512], F32)
    st = AttnState(kb)
    rl = kb.alloc([512], F32)
    ob = [kb.alloc([512], BF16) for _ in range(2)]
    oi = 0
    for typ in ("band", "mla"):
        if typ == "band":
            Dk, scale, nb = 64, 64 ** -0.5, 8
            load_qkv(kb, qT, kT, V, d["qa"], d["ka"], d["va"], 64, 64)
            kb.dma(bt, d["bb"], [], ["btiles"], "btiles")
            od = d["oa"]
        else:
            Dk, scale, nb = 96, 96 ** -0.5, 4
            load_qkv(kb, qT, kT, V, d["qm"], d["km"], d["vm"], 96, 64)
            kb.dma(bt[:, 0:4, :], d["mb"], [], ["btiles"], "btiles")
            od = d["om"]
        for qb in range(SEQ // 512):
            Q0 = qb * 512
            if typ == "band":
                klist = [(K0 // 128, (K0 - Q0 + 512) // 128) for K0 in range(Q0 - 512, Q0 + 512, 128) if K0 >= 0]
            else:
                klist = [(kbi, None) for kbi in range(Q0 // 128)] + [(Q0 // 128 + j, j) for j in range(4)]
            Ob, Lb = 3 + qb % 2, 5 + qb % 2
            attn_block(kb, st, qT[0:Dk, Q0:Q0 + 512], kT, V, Dk, 64, 0, scale, klist, bt, None, Ob, Lb, ["attQ", "attK"])
            kb.S.op("dve", lambda e, Lb=Lb: e.reciprocal(out=rl[0:64, :], in_=kb.ps[Lb][0:64, :]), ["ps%d" % Lb], ["attrl"])
            o, on = ob[oi % 2], "atto%d" % (oi % 2); oi += 1
            kb.tt("dve", o[0:64, :], kb.ps[Ob][0:64, :], rl[0:64, :], ALU.mult, ["ps%d" % Ob, "attrl"], [on])
            kb.dma(od[:, Q0:Q0 + 512], o[0:64, :], [on], [], on, q="sp")
    kb.release(m)


def emit_attn_odd(kb, d, lam_init):
    m = kb.mark()
    qT = kb.alloc([SEQ], BF16)
    kT = kb.alloc([SEQ], BF16)
    V = kb.alloc([128, 128], BF16)
    bt = kb.alloc([5, 512], F32)
    cb = kb.alloc([1], F32)
    lamt = kb.alloc([256], F32)
    lt = kb.alloc([128], F32)
    s12 = kb.alloc([2], F32)
    neglam = kb.alloc([1], F32)
    gs = kb.alloc([1], F32)
    st = AttnState(kb)
    rl = [kb.alloc([512], F32) for _ in range(2)]
    o01 = [kb.alloc([512], F32) for _ in range(2)]
    osum = kb.alloc([512], F32)
    osq = kb.alloc([512], BF16)
    inv = kb.alloc([512], F32)
    ob = [kb.alloc([512], BF16) for _ in range(2)]
    load_qkv(kb, qT, kT, V, d["qc"], d["kc"], d["vc"], 128, 128)
    kb.dma(bt, d["tb"], [], ["btiles"], "btiles")
    kb.dma(cb, d["cb"], [], ["cbias"], "cbias")
    kb.dma(lamt, d["lam"], [], ["lamt"], "lamt")
    kb.dma(gs, d["gs"], [], ["gs"], "gs")
    kb.tt("dve", lt[:, 0:64], lamt[:, 0:64], lamt[:, 64:128], ALU.mult, ["lamt"], ["lt"])
    kb.tt("dve", lt[:, 64:128], lamt[:, 128:192], lamt[:, 192:256], ALU.mult, ["lamt"], ["lt"])
    kb.S.op("dve", lambda e: e.reduce_sum(out=s12[:, 0:1], in_=lt[:, 0:64], axis=AX.X), ["lt"], ["s12"])
    kb.S.op("dve", lambda e: e.reduce_sum(out=s12[:, 1:2], in_=lt[:, 64:128], axis=AX.X), ["s12", "lt"], ["s12"])
    kb.act(s12, s12, AF.Exp, ["s12"], ["s12"])
    kb.stt("dve", neglam, s12[:, 1:2], -lam_init, s12[:, 0:1], ALU.add, ALU.subtract, ["s12"], ["neglam"])
    kb.ts("dve", gs, gs, 1.0 - lam_init, None, ALU.mult, None, ["gs"], ["gs"])
    scale = 64 ** -0.5
    oi = 0
    for qb in range(SEQ // 512):
        Q0 = qb * 512
        klist = [(kbi, None) for kbi in range(max(0, Q0 // 128 - 1))]
        klist += [(K0 // 128, (K0 - Q0 + 128) // 128) for K0 in range(Q0 - 128, Q0 + 512, 128) if K0 >= 0]
        for n in range(2):
            attn_block(kb, st, qT[n * 64:(n + 1) * 64, Q0:Q0 + 512], kT, V, 64, 128, n * 64, scale, klist, bt, cb[:, 0:1],
                       3 + n, 5 + n, ["attQ", "attK"])
        for n in range(2):
            kb.S.op("dve", lambda e, n=n: e.reciprocal(out=rl[n], in_=kb.ps[5 + n]), ["ps%d" % (5 + n)], ["attrl%d" % n])
            kb.tt("dve", o01[n], kb.ps[3 + n], rl[n], ALU.mult, ["ps%d" % (3 + n), "attrl%d" % n], ["atto01%d" % n])
        kb.stt("dve", osum, o01[1], neglam[:, 0:1], o01[0], ALU.mult, ALU.add, ["atto010", "atto011", "neglam"], ["attosum"])
        kb.act(osq, osum, AF.Square, ["attosum"], ["attosq"])
        kb.mm(kb.ps[7], [(kb.ones, osq)], ["ones", "attosq"], ["ps7"])
        kb.rstd(inv, kb.ps[7], 1.0 / 128, kb.eps_rms, ["ps7"], ["attinv"])
        kb.tt("pool", osum, osum, inv, ALU.mult, ["attosum", "attinv"], ["attosum"])
        o, on = ob[oi % 2], "atto%d" % (oi % 2); oi += 1
        kb.act(o, osum, AF.Copy, ["attosum", "gs"], [on], scale=gs[:, 0:1])
        kb.dma(d["oc"][:, Q0:Q0 + 512], o, [on], [], on, q="sp")
    kb.release(m)


def declare_attn_even(kb):
    d = {}
    for nm, shp in (("qa", [64, SEQ]), ("ka", [64, SEQ]), ("va", [SEQ, 64]), ("qm", [96, SEQ]), ("km", [96, SEQ]), ("vm", [SEQ, 64])):
        d[nm] = kb.din(nm, shp, BF16)
    d["bb"] = kb.din("bb", [128, 8, 512])
    d["mb"] = kb.din("mb", [128, 4, 512])
    d["oa"] = kb.dout("oa", [64, SEQ], BF16)
    d["om"] = kb.dout("om", [64, SEQ], BF16)
    return d


def declare_attn_odd(kb):
    d = {}
    for nm, shp in (("qc", [128, SEQ]), ("kc", [128, SEQ]), ("vc", [SEQ, 128])):
        d[nm] = kb.din(nm, shp, BF16)
    d["tb"] = kb.din("tb", [128, 5, 512])
    d["cb"] = kb.din("cb", [128, 1])
    d["lam"] = kb.din("lam", [128, 256])
    d["gs"] = kb.din("gs", [128, 1])
    d["oc"] = kb.dout("oc", [128, SEQ], BF16)
    return d


def band_tiles(rel_bias_h):
    tab = np.concatenate([rel_bias_h.astype(np.float32), np.array([NEG], np.float32)])
    kk = np.arange(128)[:, None]
    qi = np.arange(512)[None, :]
    out = np.zeros((128, 8, 512), np.float32)
    for t in range(8):
        dlt = -512 + 128 * t
        ck = (dlt + kk) // 64
        cq = qi // 64
        valid = (cq - ck >= 0) & (cq - ck <= 8)
        rel = qi - dlt - kk
        idx = np.where(valid, np.clip(rel, -128, 128) + 128, 257)
        out[:, t, :] = tab[idx]
    return out


def mla_tiles():
    kk = np.arange(128)[:, None]
    qi = np.arange(512)[None, :]
    out = np.zeros((128, 4, 512), np.float32)
    for t in range(4):
        dlt = 128 * t
        valid = ((dlt + kk) // 64) <= (qi // 64)
        out[:, t, :] = np.where(valid, 0.0, NEG)
    return out


def t5_bucket_np(rel):
    import jax
    import jax.numpy as jnp
    try:
        with jax.default_device(jax.devices("cpu")[0]):
            return _t5_bucket_impl(jnp, rel)
    except RuntimeError:
        return _t5_bucket_impl(jnp, rel)


def _t5_bucket_impl(jnp, rel):
    rel = jnp.asarray(rel)
    half = 16
    max_exact = 8
    ret = jnp.where(rel > 0, half, 0)
    n = jnp.abs(rel)
    large = max_exact + (jnp.log(jnp.maximum(n, max_exact).astype(jnp.float32) / max_exact)
                         / math.log(128 / max_exact) * (half - max_exact)).astype(jnp.int32)
    large = jnp.minimum(large, half - 1)
    return np.asarray(ret + jnp.where(n < max_exact, n, large))


def t5_tiles(t5_h):
    tab = np.concatenate([t5_h.astype(np.float32), np.array([NEG], np.float32)])
    kk = np.arange(128)[:, None]
    qi = np.arange(512)[None, :]
    out = np.zeros((128, 5, 512), np.float32)
    for t in range(5):
        dlt = -128 + 128 * t
        valid = ((dlt + kk) // 64) <= (qi // 64)
        rel = dlt + kk - qi
        idx = np.where(valid, t5_bucket_np(rel), 32)
        out[:, t, :] = tab[idx]
    return out


def ln_tokmajor(kb, x, xname, g, b, stats, mv, rs):
    for hf in range(2):
        kb.S.op("dve", lambda e, hf=hf: e.bn_stats(out=stats[:, hf, :], in_=x[:, hf * 512:(hf + 1) * 512]), [xname], ["lnstats"])
    kb.S.op("dve", lambda e: e.bn_aggr(out=mv, in_=stats), ["lnstats"], ["lnmv"])
    kb.rstd(rs, mv[:, 1:2], 1.0, kb.eps_ln, ["lnmv"], ["lnrs"])
    kb.ts("dve", x, x, mv[:, 0:1], rs[:, 0:1], ALU.subtract, ALU.mult, [xname, "lnmv", "lnrs"], [xname])
    kb.tt("pool", x, x, g, ALU.mult, [xname, "lng"], [xname])
    kb.tt("pool", x, x, b, ALU.add, [xname, "lnb"], [xname])


def emit_post(kb, d, last):
    m = kb.mark()
    G = 1024
    NS = G // 128
    ACC = kb.alloc([NS, 1024], F32)
    XT = kb.alloc([8, G], BF16)
    HT = kb.alloc([8, G], BF16)
    WO = [kb.alloc([8, 1024], BF16) for _ in range(2)]
    mk = kb.mark()
    WI = [kb.alloc([8, 256], BF16) for _ in range(4)]
    kb.release(mk)
    PW = kb.alloc([2, 1024], BF16)
    kb.off = mk + 4 * 8 * 256 * 2
    stg = [kb.alloc([2048], F32) for _ in range(3)]
    stn = ["stg0", "stg1", "stg2"]
    lng = kb.alloc([1024], F32)
    lnb = kb.alloc([1024], F32)
    wrh = kb.alloc([8, 32], BF16)
    wrl = kb.alloc([8, 32], BF16)
    rb = kb.alloc([32], F32)
    binT = kb.alloc([16, 32], F32)
    BO = kb.alloc([1024], BF16)
    gates = kb.alloc([NS, 32], F32)
    gT = kb.alloc([G], BF16)
    xs = [kb.alloc([1024], F32) for _ in range(2)]
    xh = [kb.alloc([1024], BF16) for _ in range(2)]
    xl = [kb.alloc([1024], BF16) for _ in range(2)]
    XTl = [kb.alloc([8, 128], BF16) for _ in range(2)]
    OT = [kb.alloc([8, 128], BF16) for _ in range(2)]
    pt = [kb.alloc([256], F32) for _ in range(2)]
    pb16 = [kb.alloc([256], BF16) for _ in range(2)]
    pT = [kb.alloc([2, 128], BF16) for _ in range(2)]
    gt = [kb.alloc([512], F32) for _ in range(2)]
    sig = gt
    lt = [kb.alloc([512], F32) for _ in range(2)]
    sg = [kb.alloc([512], F32) for _ in range(2)]
    stats = kb.alloc([2, 6], F32)
    mv = kb.alloc([2], F32)
    rs = kb.alloc([1], F32)
    sm = kb.alloc([64], F32)
    top8 = kb.alloc([8], F32)
    sc = kb.alloc([4], F32)
    gb = kb.alloc([32], BF16)
    bih = HT[:, 0:2, :].rearrange("p a b -> p (a b)")
    bil = HT[:, 2:4, :].rearrange("p a b -> p (a b)")
    xo16 = [kb.alloc([8, 128], BF16) for _ in range(2)]
    li = [0]

    def ldc(dst, src, shape, dname, eng):
        kb.load_cast(dst, src, shape, stg, stn, li[0], dname, eng=eng)
        li[0] += 1

    rwv = d["rw"].rearrange("(c p) e -> p c e", p=128)
    kb.dma(stg[0][:, 0:256].rearrange("p (c e) -> p c e", c=8), rwv, [], ["stg0"], "stg0"); li[0] = 1
    kb.cp("dve", wrh, stg[0][:, 0:256].rearrange("p (c e) -> p c e", c=8), ["stg0"], ["wrh"])
    kb.tt("dve", wrl, stg[0][:, 0:256].rearrange("p (c e) -> p c e", c=8), wrh, ALU.subtract, ["stg0", "wrh"], ["wrl"])
    kb.dma(rb, d["rb"], [], ["rb"], "rb", q="pool")
    kb.dma(stg[1][0:32, 0:1024], d["bo"], [], ["stg1"], "stg1"); li[0] = 2
    kb.cp("dve", BO[0:32, :], stg[1][0:32, 0:1024], ["stg1"], ["BO"])
    kb.dma(stg[2][0:32, :], d["bi"], [], ["stg2"], "stg2"); li[0] = 3
    kb.cp("dve", bih[0:32, :], stg[2][0:32, :], ["stg2"], ["HT0"])
    kb.tt("dve", bil[0:32, :], stg[2][0:32, :], bih[0:32, :], ALU.subtract, ["stg2", "HT0"], ["HT1"])
    pbk, pn = kb.bank()
    for k in range(16):
        kb.mm(pbk[:, k * 32:(k + 1) * 32], [(bih[0:32, k * 128:(k + 1) * 128], kb.ident[0:32, 0:32]),
                                           (bil[0:32, k * 128:(k + 1) * 128], kb.ident[0:32, 0:32])], ["HT0", "HT1", "ident"], [pn])
    kb.cp("dve", binT, pbk.rearrange("p (k e) -> p k e", k=16), [pn], ["binT"])

    otv = d["ot"].rearrange("(c p) t -> p c t", p=128)
    xtov = None if last else d["xto"].rearrange("(c p) t -> p c t", p=128)
    for g in range(T // G):
        for c in range(8):
            ldc(WO[0][:, c, :], d["wo"][c * 128:(c + 1) * 128, :], [1024], "WO0", ("dve", "pool")[c % 2])
        for c in range(8):
            ldc(WO[1][:, c, :], d["gw"][c * 128:(c + 1) * 128, :], [1024], "WO1", ("dve", "pool")[c % 2])
        for c in range(2):
            ldc(PW[:, c, :], d["pw"][c * 128:(c + 1) * 128, :], [1024], "WI0", "dve")
        kb.dma(lng, d["g1"], [], ["lng"], "lng", q="pool")
        kb.dma(lnb, d["b1"], [], ["lnb"], "lnb", q="pool")
        for s in range(NS):
            k = s % 2
            tg = g * G + s * 128
            xn_, xhn, xln = "xs%d" % k, "xh%d" % k, "xl%d" % k
            kb.dma(OT[k], otv[:, :, tg:tg + 128], [], ["OT%d" % k], "OT%d" % k)
            kb.dma(xs[k], d["xres"][tg:tg + 128, :], [], [xn_], xn_, q="pool")
            kb.dma(pt[k], d["p"][tg:tg + 128, :], [], ["pt%d" % k], "pt%d" % k, q="pool")
            for hf in range(2):
                pbk, pn = kb.bank()
                kb.mm(pbk, [(OT[k][:, c, :], WO[0][:, c, hf * 512:(hf + 1) * 512]) for c in range(8)], ["OT%d" % k, "WO0"], [pn])
                kb.stt("dve", xs[k][:, hf * 512:(hf + 1) * 512], xs[k][:, hf * 512:(hf + 1) * 512], ALPHA, pbk, ALU.mult, ALU.add, [xn_, pn], [xn_])
            ln_tokmajor(kb, xs[k], xn_, lng, lnb, stats, mv, rs)
            kb.cp("act", xh[k], xs[k], [xn_], [xhn])
            kb.tt("dve", xl[k], xs[k], xh[k], ALU.subtract, [xn_, xhn], [xln])
            pbk, pn = kb.bank()
            pv = pbk.bitcast(BF16)
            for c in range(8):
                kb.tr(pv[:, c * 128:(c + 1) * 128], xh[k][:, c * 128:(c + 1) * 128], kb.ident, [xhn, "ident"], [pn])
            kb.cp("act", XT[:, :, s * 128:(s + 1) * 128], pv[:, 0:1024].rearrange("p (c t) -> p c t", c=8), [pn], ["XT"])
            pbk, pn = kb.bank()
            pv = pbk.bitcast(BF16)
            for c in range(8):
                kb.tr(pv[:, c * 128:(c + 1) * 128], xl[k][:, c * 128:(c + 1) * 128], kb.ident, [xln, "ident"], [pn])
            kb.cp("dve", XTl[k], pv[:, 0:1024].rearrange("p (c t) -> p c t", c=8), [pn], ["XTl%d" % k])
            pbk, pn = kb.bank()
            prs = [(XT[:, c, s * 128:(s + 1) * 128], wrh[:, c, :]) for c in range(8)]
            prs += [(XT[:, c, s * 128:(s + 1) * 128], wrl[:, c, :]) for c in range(8)]
            prs += [(XTl[k][:, c, :], wrh[:, c, :]) for c in range(8)]
            kb.mm(pbk[:, 0:32], prs, ["XT", "XTl%d" % k, "wrh", "wrl"], [pn])
            lg, ee = sm[:, 0:32], sm[:, 32:64]
            kb.tt("dve", lg, pbk[:, 0:32], rb, ALU.add, [pn, "rb"], ["sm"])
            kb.S.op("dve", lambda e, lg=lg: e.max(out=top8, in_=lg), ["sm"], ["top8"])
            kb.ts("dve", sc[:, 0:1], top8[:, 0:1], -1.0, None, ALU.mult, None, ["top8"], ["sc"])
            kb.act(ee, lg, AF.Exp, ["sm", "sc"], ["sm2"], bias=sc[:, 0:1])
            kb.ts("dve", lg, lg, top8[:, 3:4], None, ALU.is_ge, None, ["sm", "top8"], ["sm"])
            kb.tt("dve", ee, ee, lg, ALU.mult, ["sm", "sm2"], ["sm2"])
            kb.S.op("dve", lambda e, ee=ee: e.reduce_sum(out=sc[:, 1:2], in_=ee, axis=AX.X), ["sm2"], ["sc2"])
            kb.S.op("dve", lambda e: e.reciprocal(out=sc[:, 2:3], in_=sc[:, 1:2]), ["sc2"], ["sc3"])
            kb.ts("dve", gates[:, s, :], ee, sc[:, 2:3], None, ALU.mult, None, ["sm2", "sc3"], ["gates"])
            kb.cp("dve", gb, gates[:, s, :], ["gates"], ["gb"])
            pbk, pn = kb.bank()
            pv = pbk.bitcast(BF16)
            kb.tr(pv[0:32, 0:128], gb, kb.ident, ["gb", "ident"], [pn])
            kb.cp("dve", gT[0:32, s * 128:(s + 1) * 128], pv[0:32, 0:128], [pn], ["gT"])
            kb.cp("dve", pb16[k], pt[k], ["pt%d" % k], ["pb16%d" % k])
            pbk, pn = kb.bank()
            pv = pbk.bitcast(BF16)
            for c in range(2):
                kb.tr(pv[:, c * 128:(c + 1) * 128], pb16[k][:, c * 128:(c + 1) * 128], kb.ident, ["pb16%d" % k, "ident"], [pn])
            kb.cp("dve", pT[k], pv[:, 0:256].rearrange("p (c t) -> p c t", c=2), [pn], ["pT%d" % k])
            for hf in range(2):
                hs = slice(hf * 512, (hf + 1) * 512)
                p1, pn1 = kb.bank()
                kb.mm(p1, [(XT[:, c, s * 128:(s + 1) * 128], WO[1][:, c, hs]) for c in range(8)], ["XT", "WO1"], [pn1])
                kb.act(sig[hf], p1, AF.Sigmoid, [pn1], ["gt%d" % hf])
                p2, pn2 = kb.bank()
                kb.mm(p2, [(pT[k][:, c, :], PW[:, c, hs]) for c in range(2)], ["pT%d" % k, "WI0"], [pn2])
                kb.tt("dve", sig[hf], sig[hf], p2, ALU.mult, ["gt%d" % hf, pn2], ["gt%d" % hf])
                kb.stt("dve", ACC[:, s, hs], xs[k][:, hs], ALPHA, sig[hf], ALU.mult, ALU.add, [xn_, "gt%d" % hf], ["ACC%d" % s])
                p3, pn3 = kb.bank()
                kb.mm(p3, [(gT[0:32, s * 128:(s + 1) * 128], BO[0:32, hs])], ["gT", "BO"], [pn3])
                kb.tt("dve", ACC[:, s, hs], ACC[:, s, hs], p3, ALU.add, ["ACC%d" % s, pn3], ["ACC%d" % s])
        wiv = d["wi"]
        wov = d["wout"]
        pieces = [(e, j) for e in range(NE) for j in range(8)]

        def load_wi(i):
            e, j = pieces[i]
            ldc(WI[i % 4].rearrange("p c f -> p (c f)"), wiv[e, j], [2048], "WI%d" % (i % 4), "pool")

        load_wi(0)
        load_wi(1)
        ai = 0
        for e in range(NE):
            wos = e % 2
            for j in range(8):
                i = e * 8 + j
                if i + 2 < len(pieces):
                    load_wi(i + 2)
                if j % 2 == 0:
                    mq = j // 2
                    ldc(WO[wos][:, 2 * mq:2 * mq + 2, :], wov[e, 2 * mq * 128:(2 * mq + 2) * 128, :].rearrange("(k p) f -> p k f", p=128),
                        [2, 1024], "WO%d" % wos, "act")
                w = WI[i % 4]
                wn = "WI%d" % (i % 4)
                for tt in range(2):
                    ts_ = slice(tt * 512, (tt + 1) * 512)
                    pg, png = kb.bank()
                    kb.mm(pg, [(w[:, c, 0:128], XT[:, c, ts_]) for c in range(8)], [wn, "XT"], [png])
                    pl, pnl = kb.bank()
                    kb.mm(pl, [(w[:, c, 128:256], XT[:, c, ts_]) for c in range(8)], [wn, "XT"], [pnl])
                    a = ai % 2
                    ai += 1
                    kb.ts("dve", gt[a], pg, binT[:, j, e:e + 1], 7.0, ALU.add, ALU.min, [png, "binT"], ["gt%d" % a])
                    kb.act(sg[a], gt[a], AF.Sigmoid, ["gt%d" % a], ["sg%d" % a], scale=1.702)
                    kb.ts("dve", lt[a], pl, binT[:, 8 + j, e:e + 1], 7.0, ALU.add, ALU.min, [pnl, "binT"], ["lt%d" % a])
                    kb.ts("pool", lt[a], lt[a], -7.0, 1.0, ALU.max, ALU.add, ["lt%d" % a], ["lt%d" % a])
                    kb.tt("pool", gt[a], gt[a], sg[a], ALU.mult, ["gt%d" % a, "sg%d" % a], ["gt%d" % a])
                    kb.tt("pool", HT[:, j, ts_], gt[a], lt[a], ALU.mult, ["gt%d" % a, "lt%d" % a], ["HT%d" % tt])
            for s in range(NS):
                for hf in range(2):
                    hs = slice(hf * 512, (hf + 1) * 512)
                    py, pny = kb.bank()
                    kb.mm(py, [(HT[:, kk, s * 128:(s + 1) * 128], WO[wos][:, kk, hs]) for kk in range(8)], ["HT%d" % (s // 4), "WO%d" % wos], [pny])
                    kb.stt("dve", ACC[:, s, hs], py, gates[:, s, e:e + 1], ACC[:, s, hs], ALU.mult, ALU.add, [pny, "gates", "ACC%d" % s], ["ACC%d" % s])
        kb.dma(lng, d["g2"], [], ["lng"], "lng", q="pool")
        kb.dma(lnb, d["b2"], [], ["lnb"], "lnb", q="pool")
        for s in range(NS):
            k = s % 2
            tg = g * G + s * 128
            an = "ACC%d" % s
            ln_tokmajor(kb, ACC[:, s, :], an, lng, lnb, stats, mv, rs)
            kb.dma(d["xo"][tg:tg + 128, :], ACC[:, s, :], [an], [], an, q="sp")
            if not last:
                kb.cp("act", xh[k], ACC[:, s, :], [an], ["xh%d" % k])
                pbk, pn = kb.bank()
                pv = pbk.bitcast(BF16)
                for c in range(8):
                    kb.tr(pv[:, c * 128:(c + 1) * 128], xh[k][:, c * 128:(c + 1) * 128], kb.ident, ["xh%d" % k, "ident"], [pn])
                kb.cp("dve", xo16[k], pv[:, 0:1024].rearrange("p (c t) -> p c t", c=8), [pn], ["xo16%d" % k])
                kb.dma(xtov[:, :, tg:tg + 128], xo16[k], ["xo16%d" % k], [], "xo16%d" % k, q="pool")
    kb.release(m)


def declare_post(kb, last):
    d = {}
    d["ot"] = kb.din("ot", [1024, T], BF16)
    d["xres"] = kb.din("xres", [T, 1024])
    d["p"] = kb.din("p", [T, 256])
    d["wo"] = kb.din("wo", [1024, 1024])
    d["gw"] = kb.din("gw", [1024, 1024])
    d["pw"] = kb.din("pw", [256, 1024])
    for nm in ("g1", "b1", "g2", "b2"):
        d[nm] = kb.din(nm, [128, 1024])
    d["rw"] = kb.din("rw", [1024, 32])
    d["rb"] = kb.din("rb", [128, 32])
    d["bo"] = kb.din("bo", [32, 1024])
    d["bi"] = kb.din("bi", [32, 2048])
    d["wi"] = kb.din("wi", [32, 8, 128, 2048])
    d["wout"] = kb.din("wout", [32, 1024, 1024])
    d["xo"] = kb.dout("xo", [T, 1024])
    if not last:
        d["xto"] = kb.dout("xto", [1024, T], BF16)
    return d


def bc128(v):
    return np.ascontiguousarray(np.broadcast_to(np.asarray(v, np.float32).reshape(1, -1), (128, v.size)))


def prep_post(i, inp, wo):
    wi = inp["exp_w_in"][i]
    w = wi.reshape(32, 8, 128, 2, 8, 128).transpose(0, 4, 2, 1, 3, 5).reshape(32, 8, 128, 2048)
    return dict(wo=np.ascontiguousarray(wo), gw=np.ascontiguousarray(inp["ple_gate_w"][i]), pw=np.ascontiguousarray(inp["ple_proj_w"][i]),
                g1=bc128(inp["ln_mix_g"][i]), b1=bc128(inp["ln_mix_b"][i]), g2=bc128(inp["ln_ffn_g"][i]), b2=bc128(inp["ln_ffn_b"][i]),
                rw=np.ascontiguousarray(inp["router_w"][i]), rb=bc128(inp["router_b"][i]),
                bo=np.ascontiguousarray(inp["exp_b_out"][i]), bi=np.ascontiguousarray(inp["exp_b_in"][i]),
                wi=np.ascontiguousarray(w), wout=np.ascontiguousarray(inp["exp_w_out"][i]))


def _lam_init(i):
    return 0.8 - 0.6 * math.exp(-0.3 * i)


def _cat_heads_fm(outs, key, hd):
    return [np.ascontiguousarray(np.concatenate([outs[r][key][h * hd:(h + 1) * hd, :] for r in range(NCORES)], axis=1)) for h in range(8)]


def _cat_heads_tm(outs, key, hd):
    return [np.ascontiguousarray(np.concatenate([outs[r][key][:, h * hd:(h + 1) * hd] for r in range(NCORES)], axis=0)) for h in range(8)]


def kernel(**inp):
    inp = {k: np.asarray(v) for k, v in inp.items()}
    x = inp["x"][0]
    pos_tabs = [rope_tables(np.arange(r * T, (r + 1) * T)) for r in range(NCORES)]
    mb = mla_tiles()

    def pre_even_maps(j):
        pe = prep_even(inp["ab_w_in"][j], inp["ab_q_norm"][j], inp["ab_kv_norm"][j], inp["ab_w_uq"][j], inp["ab_w_ukv"][j])
        maps = []
        for r in range(NCORES):
            m = dict(pe)
            cq, sq, ck, sk = pos_tabs[r]
            m.update(cq=cq, sq=sq, ck=ck, sk=sk)
            maps.append(m)
        return maps

    def attn_even(outs, j):
        kb = KB(); kb.consts()
        d = declare_attn_even(kb)
        emit_attn_even(kb, d)
        qa = _cat_heads_fm(outs, "qa", 64); ka = _cat_heads_fm(outs, "ka", 64); va = _cat_heads_tm(outs, "va", 64)
        qm = _cat_heads_fm(outs, "qm", 96); km = _cat_heads_fm(outs, "km", 96); vm = _cat_heads_tm(outs, "vm", 64)
        maps = [dict(qa=qa[h], ka=ka[h], va=va[h], qm=qm[h], km=km[h], vm=vm[h],
                     bb=band_tiles(inp["ab_rel_bias"][j][:, h]), mb=mb) for h in range(8)]
        res = run(kb, maps)
        ots = []
        for r in range(NCORES):
            cs = slice(r * T, (r + 1) * T)
            ots.append(np.ascontiguousarray(np.concatenate([res[h]["oa"][:, cs] for h in range(8)] + [res[h]["om"][:, cs] for h in range(8)], axis=0)))
        return ots

    def attn_odd(outs, j, i):
        kb = KB(); kb.consts()
        d = declare_attn_odd(kb)
        emit_attn_odd(kb, d, _lam_init(i))
        qc = _cat_heads_fm(outs, "qc", 128); kc = _cat_heads_fm(outs, "kc", 128); vc = _cat_heads_tm(outs, "vc", 128)
        lam = np.ascontiguousarray(np.broadcast_to(inp["c_lambda"][j].reshape(1, 256), (128, 256)))
        gs = np.ascontiguousarray(inp["c_subln"][j].reshape(128, 1))
        maps = [dict(qc=qc[h], kc=kc[h], vc=vc[h], tb=t5_tiles(inp["t5_table"][:, h]),
                     cb=np.full((128, 1), inp["t5_table"][15, h], np.float32), lam=lam, gs=gs) for h in range(8)]
        res = run(kb, maps)
        return [np.ascontiguousarray(np.concatenate([res[h]["oc"][:, r * T:(r + 1) * T] for h in range(8)], axis=0)) for r in range(NCORES)]

    kb = KB(); kb.consts()
    xin = kb.din("x", [T, 1024])
    xts = kb.dout("xts", [1024, T], BF16)
    emit_x2xt(kb, xin, xts, T)
    kb.S.barrier()
    d = declare_pre_even(kb)
    d["xt"] = xts
    emit_pre_even(kb, d)
    maps = pre_even_maps(0)
    for r in range(NCORES):
        maps[r]["x"] = np.ascontiguousarray(x[r * T:(r + 1) * T])
        maps[r]["xt"] = np.zeros((1024, T), NPBF)
    outs = run(kb, maps)
    xres = [np.ascontiguousarray(x[r * T:(r + 1) * T]) for r in range(NCORES)]

    for i in range(DEPTH):
        j = i // 2
        if i % 2 == 0:
            ots = attn_even(outs, j)
            wo = inp["ab_w_out"][j]
        else:
            ots = attn_odd(outs, j, i)
            wo = inp["c_w_out"][j]
        last = i == DEPTH - 1
        kb = KB(); kb.consts()
        dA = {}
        dA["ot"] = kb.din("ot", [1024, T], BF16)
        dA["xres"] = kb.din("xres", [T, 1024])
        dA["p"] = kb.din("p", [T, 256])
        dA["wo"] = kb.din("wo", [1024, 1024])
        dA["gw"] = kb.din("gw", [1024, 1024])
        dA["pw"] = kb.din("pw", [256, 1024])
        dA["g1"] = kb.din("g1", [128, 1024])
        dA["b1"] = kb.din("b1", [128, 1024])
        dA["rw"] = kb.din("rw", [1024, 32])
        dA["rb"] = kb.din("rb", [128, 32])
        dA["bo"] = kb.din("bo", [32, 1024])
        dA["xt1"] = kb.dout("xt1", [1024, T], BF16)
        dA["gates"] = kb.dout("gates", [T, 32])
        dA["base"] = kb.dout("base", [T, 1024])
        emit_postA(kb, dA)
        cm = dict(wo=np.ascontiguousarray(wo), gw=np.ascontiguousarray(inp["ple_gate_w"][i]), pw=np.ascontiguousarray(inp["ple_proj_w"][i]),
                  g1=bc128(inp["ln_mix_g"][i]), b1=bc128(inp["ln_mix_b"][i]), rw=np.ascontiguousarray(inp["router_w"][i]),
                  rb=bc128(inp["router_b"][i]), bo=np.ascontiguousarray(inp["exp_b_out"][i]))
        maps = []
        for r in range(NCORES):
            mm_ = dict(cm)
            mm_["ot"] = ots[r]; mm_["xres"] = xres[r]
            mm_["p"] = np.ascontiguousarray(inp["p"][i, 0, r * T:(r + 1) * T])
            maps.append(mm_)
        outsA = run(kb, maps)
        kb = KB(); kb.consts()
        dM = {}
        dM["xt"] = kb.din("xt", [1024, SEQ], BF16)
        dM["gates"] = kb.din("gates", [SEQ, 4])
        dM["wi"] = kb.din("wi", [4, 8, 128, 2048])
        dM["wout"] = kb.din("wout", [4, 1024, 1024])
        dM["bi"] = kb.din("bi", [4, 2048])
        dM["part"] = kb.dout("part", [SEQ, 1024])
        emit_moe(kb, dM, 4)
        xt_all = np.ascontiguousarray(np.concatenate([outsA[r]["xt1"] for r in range(NCORES)], axis=1))
        g_all = np.concatenate([outsA[r]["gates"] for r in range(NCORES)], axis=0)
        wi_full = inp["exp_w_in"][i]
        maps = []
        for c in range(NCORES):
            es = slice(4 * c, 4 * c + 4)
            w = wi_full[es].reshape(4, 8, 128, 2, 8, 128).transpose(0, 4, 2, 1, 3, 5).reshape(4, 8, 128, 2048)
            maps.append(dict(xt=xt_all, gates=np.ascontiguousarray(g_all[:, es]), wi=np.ascontiguousarray(w),
                             wout=np.ascontiguousarray(inp["exp_w_out"][i][es]), bi=np.ascontiguousarray(inp["exp_b_in"][i][es])))
        outsM = run(kb, maps)
        kb = KB(); kb.consts()
        d = {}
        d["base"] = kb.din("base", [T, 1024])
        d["parts"] = kb.din("parts", [NCORES, T, 1024])
        d["g2"] = kb.din("g2", [128, 1024])
        d["b2"] = kb.din("b2", [128, 1024])
        d["xo"] = kb.dout("xo", [T, 1024])
        if not last:
            d["xto"] = kb.dout("xto", [1024, T], BF16)
        emit_postC(kb, d, last)
        g2 = bc128(inp["ln_ffn_g"][i]); b2 = bc128(inp["ln_ffn_b"][i])
        maps = []
        for r in range(NCORES):
            parts = np.ascontiguousarray(np.stack([outsM[c]["part"][r * T:(r + 1) * T] for c in range(NCORES)], axis=0))
            maps.append(dict(base=outsA[r]["base"], parts=parts, g2=g2, b2=b2))
        if not last:
            kb.S.barrier()
            if (i + 1) % 2 == 0:
                d2 = declare_pre_even(kb)
                d2["xt"] = d["xto"]
                emit_pre_even(kb, d2)
                pm = pre_even_maps((i + 1) // 2)
                for r in range(NCORES):
                    maps[r].update(pm[r])
                    maps[r]["xt"] = np.zeros((1024, T), NPBF)
            else:
                d2 = {"xt": d["xto"], "w": kb.din("w", [1024, 3072])}
                for nm, shp in (("qc", [1024, T]), ("kc", [1024, T]), ("vc", [T, 1024])):
                    d2[nm] = kb.dout(nm, shp, BF16)
                emit_pre_odd(kb, d2)
                cw = np.ascontiguousarray(inp["c_w_in"][(i + 1) // 2])
                for r in range(NCORES):
                    maps[r]["w"] = cw
        outs = run(kb, maps)
        xres = [outs[r]["xo"] for r in range(NCORES)]
    out = np.concatenate([np.asarray(xres[r], np.float32) for r in range(NCORES)], axis=0)
    return out.reshape(1, SEQ, D).astype(np.float32)


def emit_postA(kb, d):
    m = kb.mark()
    NS = T // 128
    WOm = kb.alloc([8, 1024], BF16)
    GW = kb.alloc([8, 1024], BF16)
    PW = kb.alloc([2, 1024], BF16)
    stg = [kb.alloc([2048], F32) for _ in range(3)]
    stn = ["stg0", "stg1", "stg2"]
    lng = kb.alloc([1024], F32)
    lnb = kb.alloc([1024], F32)
    wrh = kb.alloc([8, 32], BF16)
    wrl = kb.alloc([8, 32], BF16)
    rb = kb.alloc([32], F32)
    BO = kb.alloc([1024], BF16)
    gates = [kb.alloc([32], F32) for _ in range(2)]
    gT = [kb.alloc([128], BF16) for _ in range(2)]
    xs = [kb.alloc([1024], F32) for _ in range(2)]
    acc = [kb.alloc([1024], F32) for _ in range(2)]
    xh = [kb.alloc([1024], BF16) for _ in range(2)]
    xl = [kb.alloc([1024], BF16) for _ in range(2)]
    XTh = [kb.alloc([8, 128], BF16) for _ in range(2)]
    XTl = [kb.alloc([8, 128], BF16) for _ in range(2)]
    OT = [kb.alloc([8, 128], BF16) for _ in range(2)]
    pt = [kb.alloc([256], F32) for _ in range(2)]
    pb16 = [kb.alloc([256], BF16) for _ in range(2)]
    pT = [kb.alloc([2, 128], BF16) for _ in range(2)]
    sig = [kb.alloc([512], F32) for _ in range(2)]
    stats = kb.alloc([2, 6], F32)
    mv = kb.alloc([2], F32)
    rs = kb.alloc([1], F32)
    sm = kb.alloc([64], F32)
    top8 = kb.alloc([8], F32)
    sc = kb.alloc([4], F32)
    gb = kb.alloc([32], BF16)
    li = [0]

    def ldc(dst, src, shape, dname, eng):
        kb.load_cast(dst, src, shape, stg, stn, li[0], dname, eng=eng)
        li[0] += 1

    rwv = d["rw"].rearrange("(c p) e -> p c e", p=128)
    s0 = stg[0][:, 0:256].rearrange("p (c e) -> p c e", c=8)
    kb.dma(s0, rwv, [], ["stg0"], "stg0"); li[0] = 1
    kb.cp("dve", wrh, s0, ["stg0"], ["wrh"])
    kb.tt("dve", wrl, s0, wrh, ALU.subtract, ["stg0", "wrh"], ["wrl"])
    kb.dma(rb, d["rb"], [], ["rb"], "rb", q="pool")
    kb.dma(stg[1][0:32, 0:1024], d["bo"], [], ["stg1"], "stg1"); li[0] = 2
    kb.cp("dve", BO[0:32, :], stg[1][0:32, 0:1024], ["stg1"], ["BO"])
    for c in range(8):
        ldc(WOm[:, c, :], d["wo"][c * 128:(c + 1) * 128, :], [1024], "WOm", ("dve", "pool")[c % 2])
    for c in range(8):
        ldc(GW[:, c, :], d["gw"][c * 128:(c + 1) * 128, :], [1024], "GW", ("dve", "pool")[c % 2])
    for c in range(2):
        ldc(PW[:, c, :], d["pw"][c * 128:(c + 1) * 128, :], [1024], "PW", "dve")
    kb.dma(lng, d["g1"], [], ["lng"], "lng", q="pool")
    kb.dma(lnb, d["b1"], [], ["lnb"], "lnb", q="pool")
    otv = d["ot"].rearrange("(c p) t -> p c t", p=128)
    xt1v = d["xt1"].rearrange("(c p) t -> p c t", p=128)
    for s in range(NS):
        k = s % 2
        tg = s * 128
        xn_, xhn, xln = "xs%d" % k, "xh%d" % k, "xl%d" % k
        kb.dma(OT[k], otv[:, :, tg:tg + 128], [], ["OT%d" % k], "OT%d" % k)
        kb.dma(xs[k], d["xres"][tg:tg + 128, :], [], [xn_], xn_, q="pool")
        kb.dma(pt[k], d["p"][tg:tg + 128, :], [], ["pt%d" % k], "pt%d" % k, q="pool")
        for hf in range(2):
            pbk, pn = kb.bank()
            kb.mm(pbk, [(OT[k][:, c, :], WOm[:, c, hf * 512:(hf + 1) * 512]) for c in range(8)], ["OT%d" % k, "WOm"], [pn])
            kb.stt("dve", xs[k][:, hf * 512:(hf + 1) * 512], xs[k][:, hf * 512:(hf + 1) * 512], ALPHA, pbk, ALU.mult, ALU.add, [xn_, pn], [xn_])
        ln_tokmajor(kb, xs[k], xn_, lng, lnb, stats, mv, rs)
        kb.cp("act", xh[k], xs[k], [xn_], [xhn])
        kb.tt("dve", xl[k], xs[k], xh[k], ALU.subtract, [xn_, xhn], [xln])
        pbk, pn = kb.bank()
        pv = pbk.bitcast(BF16)
        for c in range(8):
            kb.tr(pv[:, c * 128:(c + 1) * 128], xh[k][:, c * 128:(c + 1) * 128], kb.ident, [xhn, "ident"], [pn])
        kb.cp("act", XTh[k], pv[:, 0:1024].rearrange("p (c t) -> p c t", c=8), [pn], ["XTh%d" % k])
        kb.dma(xt1v[:, :, tg:tg + 128], XTh[k], ["XTh%d" % k], [], "XTh%d" % k, q="sp")
        pbk, pn = kb.bank()
        pv = pbk.bitcast(BF16)
        for c in range(8):
            kb.tr(pv[:, c * 128:(c + 1) * 128], xl[k][:, c * 128:(c + 1) * 128], kb.ident, [xln, "ident"], [pn])
        kb.cp("dve", XTl[k], pv[:, 0:1024].rearrange("p (c t) -> p c t", c=8), [pn], ["XTl%d" % k])
        pbk, pn = kb.bank()
        prs = [(XTh[k][:, c, :], wrh[:, c, :]) for c in range(8)]
        prs += [(XTh[k][:, c, :], wrl[:, c, :]) for c in range(8)]
        prs += [(XTl[k][:, c, :], wrh[:, c, :]) for c in range(8)]
        kb.mm(pbk[:, 0:32], prs, ["XTh%d" % k, "XTl%d" % k, "wrh", "wrl"], [pn])
        lg, ee = sm[:, 0:32], sm[:, 32:64]
        gn = "gates%d" % k
        kb.tt("dve", lg, pbk[:, 0:32], rb, ALU.add, [pn, "rb"], ["sm"])
        kb.S.op("dve", lambda e, lg=lg: e.max(out=top8, in_=lg), ["sm"], ["top8"])
        kb.ts("dve", sc[:, 0:1], top8[:, 0:1], -1.0, None, ALU.mult, None, ["top8"], ["sc"])
        kb.act(ee, lg, AF.Exp, ["sm", "sc"], ["sm2"], bias=sc[:, 0:1])
        kb.ts("dve", lg, lg, top8[:, 3:4], None, ALU.is_ge, None, ["sm", "top8"], ["sm"])
        kb.tt("dve", ee, ee, lg, ALU.mult, ["sm", "sm2"], ["sm2"])
        kb.S.op("dve", lambda e, ee=ee: e.reduce_sum(out=sc[:, 1:2], in_=ee, axis=AX.X), ["sm2"], ["sc2"])
        kb.S.op("dve", lambda e: e.reciprocal(out=sc[:, 2:3], in_=sc[:, 1:2]), ["sc2"], ["sc3"])
        kb.ts("dve", gates[k], ee, sc[:, 2:3], None, ALU.mult, None, ["sm2", "sc3"], [gn])
        kb.dma(d["gates"][tg:tg + 128, :], gates[k], [gn], [], gn, q="sp")
        kb.cp("dve", gb, gates[k], [gn], ["gb"])
        pbk, pn = kb.bank()
        pv = pbk.bitcast(BF16)
        kb.tr(pv[0:32, 0:128], gb, kb.ident, ["gb", "ident"], [pn])
        kb.cp("dve", gT[k][0:32, :], pv[0:32, 0:128], [pn], ["gT%d" % k])
        kb.cp("dve", pb16[k], pt[k], ["pt%d" % k], ["pb16%d" % k])
        pbk, pn = kb.bank()
        pv = pbk.bitcast(BF16)
        for c in range(2):
            kb.tr(pv[:, c * 128:(c + 1) * 128], pb16[k][:, c * 128:(c + 1) * 128], kb.ident, ["pb16%d" % k, "ident"], [pn])
        kb.cp("dve", pT[k], pv[:, 0:256].rearrange("p (c t) -> p c t", c=2), [pn], ["pT%d" % k])
        an = "acc%d" % k
        for hf in range(2):
            hs = slice(hf * 512, (hf + 1) * 512)
            p1, pn1 = kb.bank()
            kb.mm(p1, [(XTh[k][:, c, :], GW[:, c, hs]) for c in range(8)], ["XTh%d" % k, "GW"], [pn1])
            kb.act(sig[hf], p1, AF.Sigmoid, [pn1], ["sig%d" % hf])
            p2, pn2 = kb.bank()
            kb.mm(p2, [(pT[k][:, c, :], PW[:, c, hs]) for c in range(2)], ["pT%d" % k, "PW"], [pn2])
            kb.tt("dve", sig[hf], sig[hf], p2, ALU.mult, ["sig%d" % hf, pn2], ["sig%d" % hf])
            kb.stt("dve", acc[k][:, hs], xs[k][:, hs], ALPHA, sig[hf], ALU.mult, ALU.add, [xn_, "sig%d" % hf], [an])
            p3, pn3 = kb.bank()
            kb.mm(p3, [(gT[k][0:32, :], BO[0:32, hs])], ["gT%d" % k, "BO"], [pn3])
            kb.tt("dve", acc[k][:, hs], acc[k][:, hs], p3, ALU.add, [an, pn3], [an])
        kb.dma(d["base"][tg:tg + 128, :], acc[k], [an], [], an, q="pool")
    kb.release(m)


def emit_moe(kb, d, NEL=4):
    m = kb.mark()
    G = 1024
    NS = G // 128
    ACC = kb.alloc([NS, 1024], F32)
    XT = [kb.alloc([8, G], BF16) for _ in range(2)]
    HT = kb.alloc([8, G], BF16)
    WO = [kb.alloc([8, 1024], BF16) for _ in range(2)]
    WI = [kb.alloc([8, 256], BF16) for _ in range(4)]
    stg = [kb.alloc([2048], F32) for _ in range(3)]
    stn = ["stg0", "stg1", "stg2"]
    binT = kb.alloc([16, NEL], F32)
    gates = [kb.alloc([NS, NEL], F32) for _ in range(2)]
    bih = kb.alloc([2048], BF16)
    bil = kb.alloc([2048], BF16)
    gt = [kb.alloc([512], F32) for _ in range(2)]
    lt = [kb.alloc([512], F32) for _ in range(2)]
    sg = [kb.alloc([512], F32) for _ in range(2)]
    li = [0]

    def ldc(dst, src, shape, dname, eng):
        kb.load_cast(dst, src, shape, stg, stn, li[0], dname, eng=eng)
        li[0] += 1

    kb.dma(stg[2][0:NEL, :], d["bi"], [], ["stg2"], "stg2"); li[0] = 3
    kb.cp("dve", bih[0:NEL, :], stg[2][0:NEL, :], ["stg2"], ["bih"])
    kb.tt("dve", bil[0:NEL, :], stg[2][0:NEL, :], bih[0:NEL, :], ALU.subtract, ["stg2", "bih"], ["bil"])
    pbk, pn = kb.bank()
    for k in range(16):
        kb.mm(pbk[:, k * NEL:(k + 1) * NEL], [(bih[0:NEL, k * 128:(k + 1) * 128], kb.ident[0:NEL, 0:NEL]),
                                             (bil[0:NEL, k * 128:(k + 1) * 128], kb.ident[0:NEL, 0:NEL])], ["bih", "bil", "ident"], [pn])
    kb.cp("dve", binT, pbk[:, 0:16 * NEL].rearrange("p (k e) -> p k e", k=16), [pn], ["binT"])
    xtv = d["xt"].rearrange("(c p) t -> p c t", p=128)
    wiv, wov = d["wi"], d["wout"]
    NG = SEQ // G
    pieces = [(g, e, j) for g in range(NG) for e in range(NEL) for j in range(8)]

    def load_wi(i):
        g, e, j = pieces[i]
        ldc(WI[i % 4].rearrange("p c f -> p (c f)"), wiv[e, j], [2048], "WI%d" % (i % 4), "pool")

    load_wi(0)
    load_wi(1)
    ai = 0
    for g in range(NG):
        xk = g % 2
        xn = "XT%d" % xk
        gn = "gates%d" % xk
        kb.dma(XT[xk], xtv[:, :, g * G:(g + 1) * G], [], [xn], xn, q="sp")
        kb.dma(gates[xk], d["gates"][g * G:(g + 1) * G, :].rearrange("(s p) e -> p s e", p=128), [], [gn], gn, q="sp")
        for e in range(NEL):
            wos = (g * NEL + e) % 2
            for j in range(8):
                i = (g * NEL + e) * 8 + j
                if i + 2 < len(pieces):
                    load_wi(i + 2)
                if j % 2 == 0:
                    mq = j // 2
                    ldc(WO[wos][:, 2 * mq:2 * mq + 2, :], wov[e, 2 * mq * 128:(2 * mq + 2) * 128, :].rearrange("(k p) f -> p k f", p=128),
                        [2, 1024], "WO%d" % wos, "act")
                w = WI[i % 4]
                wn = "WI%d" % (i % 4)
                for tt in range(2):
                    ts_ = slice(tt * 512, (tt + 1) * 512)
                    pg, png = kb.bank()
                    kb.mm(pg, [(w[:, c, 0:128], XT[xk][:, c, ts_]) for c in range(8)], [wn, xn], [png])
                    pl, pnl = kb.bank()
                    kb.mm(pl, [(w[:, c, 128:256], XT[xk][:, c, ts_]) for c in range(8)], [wn, xn], [pnl])
                    a = ai % 2
                    ai += 1
                    kb.ts("dve", gt[a], pg, binT[:, j, e:e + 1], 7.0, ALU.add, ALU.min, [png, "binT"], ["gt%d" % a])
                    kb.act(sg[a], gt[a], AF.Sigmoid, ["gt%d" % a], ["sg%d" % a], scale=1.702)
                    kb.ts("dve", lt[a], pl, binT[:, 8 + j, e:e + 1], 7.0, ALU.add, ALU.min, [pnl, "binT"], ["lt%d" % a])
                    kb.ts("pool", lt[a], lt[a], -7.0, 1.0, ALU.max, ALU.add, ["lt%d" % a], ["lt%d" % a])
                    kb.tt("pool", gt[a], gt[a], sg[a], ALU.mult, ["gt%d" % a, "sg%d" % a], ["gt%d" % a])
                    kb.tt("pool", HT[:, j, ts_], gt[a], lt[a], ALU.mult, ["gt%d" % a, "lt%d" % a], ["HT%d" % tt])
            for s in range(NS):
                for hf in range(2):
                    hs = slice(hf * 512, (hf + 1) * 512)
                    py, pny = kb.bank()
                    kb.mm(py, [(HT[:, kk, s * 128:(s + 1) * 128], WO[wos][:, kk, hs]) for kk in range(8)], ["HT%d" % (s // 4), "WO%d" % wos], [pny])
                    an = "ACC%d" % s
                    if e == 0:
                        kb.ts("dve", ACC[:, s, hs], py, gates[xk][:, s, e:e + 1], None, ALU.mult, None, [pny, gn], [an])
                    else:
                        kb.stt("dve", ACC[:, s, hs], py, gates[xk][:, s, e:e + 1], ACC[:, s, hs], ALU.mult, ALU.add, [pny, gn, an], [an])
        for s in range(NS):
            kb.dma(d["part"][g * G + s * 128:g * G + (s + 1) * 128, :], ACC[:, s, :], ["ACC%d" % s], [], "ACC%d" % s, q="sp")
    kb.release(m)


def emit_postC(kb, d, last):
    m = kb.mark()
    lng = kb.alloc([1024], F32)
    lnb = kb.alloc([1024], F32)
    acc = [kb.alloc([1024], F32) for _ in range(2)]
    pin = [kb.alloc([1024], F32) for _ in range(4)]
    xh = [kb.alloc([1024], BF16) for _ in range(2)]
    xo16 = [kb.alloc([8, 128], BF16) for _ in range(2)]
    stats = kb.alloc([2, 6], F32)
    mv = kb.alloc([2], F32)
    rs = kb.alloc([1], F32)
    kb.dma(lng, d["g2"], [], ["lng"], "lng", q="pool")
    kb.dma(lnb, d["b2"], [], ["lnb"], "lnb", q="pool")
    xtov = None if last else d["xto"].rearrange("(c p) t -> p c t", p=128)
    pi = 0
    for s in range(T // 128):
        k = s % 2
        tg = s * 128
        an = "cacc%d" % k
        kb.dma(acc[k], d["base"][tg:tg + 128, :], [], [an], an, q="sp")
        for r in range(NCORES):
            b = pi % 4
            pi += 1
            kb.dma(pin[b], d["parts"][r, tg:tg + 128, :], [], ["pin%d" % b], "pin%d" % b, q=("sp", "pool")[r % 2])
            kb.tt(("dve", "pool")[r % 2], acc[k], acc[k], pin[b], ALU.add, [an, "pin%d" % b], [an])
        ln_tokmajor(kb, acc[k], an, lng, lnb, stats, mv, rs)
        kb.dma(d["xo"][tg:tg + 128, :], acc[k], [an], [], an, q="sp")
        if not last:
            kb.cp("act", xh[k], acc[k], [an], ["cxh%d" % k])
            pbk, pn = kb.bank()
            pv = pbk.bitcast(BF16)
            for c in range(8):
                kb.tr(pv[:, c * 128:(c + 1) * 128], xh[k][:, c * 128:(c + 1) * 128], kb.ident, ["cxh%d" % k, "ident"], [pn])
            kb.cp("dve", xo16[k], pv[:, 0:1024].rearrange("p (c t) -> p c t", c=8), [pn], ["xo16%d" % k])
            kb.dma(xtov[:, :, tg:tg + 128], xo16[k], ["xo16%d" % k], [], "xo16%d" % k, q="pool")
    kb.release(m)
```
